# Optimizing a Trainium2 kernel written in Bass

```python
import jax
import jax.numpy as jnp
from jax import lax
import numpy as np

D_MODEL = 1024
BATCH = 4
SEQ = 8192
DEPTH = 2

D_ATTN = D_MODEL // 2
N_ATTN_HEADS = 8
HEAD_DIM = D_ATTN // N_ATTN_HEADS
N_KV = 2
HEADS_PER_KV = N_ATTN_HEADS // N_KV
D_REC = D_MODEL - D_ATTN
N_REC_BLOCKS = 8
REC_BW = D_REC // N_REC_BLOCKS
L_CMP = 32
STRIDE = 16
L_SLC = 64
N_SEL = 16
N_LOCAL = 2
W_WIN = 512
CMP_HID = 2 * HEAD_DIM
NSA_QB = 64
R_CMP = L_CMP // STRIDE
R_SLC = L_SLC // STRIDE
N_GATES = 3
CONV_W = 4
C_LRU = 8.0
D_FF = 2816
N_EXPERTS = 8
TOP_K = 2
D_FF_EXPERT = 3584
MOE_BLOCK = 256
N_DENSE = (DEPTH + 1) // 2
N_MOE = DEPTH // 2
PLE_DIM = 256
RMS_EPS = 1e-6
NEG_INF = -1e30
KV_W = N_KV * HEAD_DIM
IN_SPLITS = (D_ATTN, KV_W, KV_W, KV_W, KV_W, KV_W, KV_W, N_ATTN_HEADS * N_GATES, D_REC, D_REC)
IN_COLS = sum(IN_SPLITS)

kernel_name = 'hybrid_nsa_rglru_moe_ple'


def rmsnorm(x, g):
    x32 = x.astype(jnp.float32)
    y = x32 * lax.rsqrt(jnp.mean(x32 * x32, axis=-1, keepdims=True) + RMS_EPS)
    return (y * g.astype(jnp.float32)).astype(x.dtype)


def alibi_slopes(n):
    return 2.0 ** (-8.0 * jnp.arange(1, n + 1, dtype=jnp.float32) / n)


def masked_softmax(s, mask):
    s = jnp.where(mask, s, NEG_INF)
    e = jnp.where(mask, jnp.exp(s - jnp.max(s, axis=-1, keepdims=True)), 0.0)
    return e / jnp.maximum(jnp.sum(e, axis=-1, keepdims=True), 1e-30)


def compress_blocks(kv, pos, w1, w2):
    b_, g_, t_, _ = kv.shape
    n_chunk = t_ // STRIDE
    n_cmp = n_chunk - R_CMP + 1
    chunks = kv.reshape(b_, g_, n_chunk, STRIDE, HEAD_DIM)
    blocks = jnp.concatenate([chunks[:, :, j:j + n_cmp] for j in range(R_CMP)], axis=3)
    flat = (blocks + pos).reshape(b_, g_, n_cmp, L_CMP * HEAD_DIM)
    return jax.nn.gelu(flat @ w1) @ w2


def nsa_attention(q, k_cmp, v_cmp, k_slc, v_slc, k_win, v_win, gates, cmp_pos, cmp_w1, cmp_w2):
    b_, t_ = q.shape[0], q.shape[1]
    n_qb = t_ // NSA_QB
    n_slc = t_ // L_SLC
    n_chunk = t_ // STRIDE
    k_top = min(N_SEL, n_slc)
    scale = HEAD_DIM ** -0.5
    grp = lambda a: a.transpose(0, 2, 1, 3)
    kc = compress_blocks(grp(k_cmp), cmp_pos[0], cmp_w1[0], cmp_w2[0])
    vc = compress_blocks(grp(v_cmp), cmp_pos[1], cmp_w1[1], cmp_w2[1])
    n_cmp = kc.shape[2]
    cmp_end = jnp.arange(n_cmp) * STRIDE + (L_CMP - 1)
    ks_blocks = grp(k_slc).reshape(b_, N_KV, n_slc, L_SLC, HEAD_DIM)
    vs_blocks = grp(v_slc).reshape(b_, N_KV, n_slc, L_SLC, HEAD_DIM)
    win_pad = ((0, 0), (0, 0), (W_WIN, 0), (0, 0))
    kw_pad = jnp.pad(grp(k_win), win_pad)
    vw_pad = jnp.pad(grp(v_win), win_pad)
    qg = q.reshape(b_, t_, N_KV, HEADS_PER_KV, HEAD_DIM).transpose(0, 2, 3, 1, 4)
    gg = jax.nn.sigmoid(gates.astype(jnp.float32)).reshape(b_, t_, N_KV, HEADS_PER_KV, N_GATES).transpose(0, 2, 3, 1, 4)
    q_blocks = jnp.moveaxis(qg.reshape(b_, N_KV, HEADS_PER_KV, n_qb, NSA_QB, HEAD_DIM), 3, 0)
    g_blocks = jnp.moveaxis(gg.reshape(b_, N_KV, HEADS_PER_KV, n_qb, NSA_QB, N_GATES), 3, 0)
    slopes = alibi_slopes(N_ATTN_HEADS).reshape(N_KV, HEADS_PER_KV, 1, 1)
    gather_blocks = jax.vmap(jax.vmap(lambda blocks, idx: blocks[idx]))
    blk_ids = jnp.arange(n_slc)
    win_offsets = jnp.arange(W_WIN + NSA_QB)

    def block_fn(args):
        qb, gb, qi = args
        s0 = qi * NSA_QB
        t = s0 + jnp.arange(NSA_QB)
        dist_c = t[:, None] - cmp_end[None, :]
        s_c = jnp.einsum('bghqd,bgcd->bghqc', qb, kc).astype(jnp.float32) * scale - slopes * dist_c.astype(jnp.float32)
        p_c = masked_softmax(s_c, dist_c >= 0)
        o_c = jnp.einsum('bghqc,bgcd->bghqd', p_c.astype(vc.dtype), vc)
        p_grp = jnp.pad(jnp.sum(p_c, axis=2), ((0, 0), (0, 0), (0, 0), (R_CMP - 1, R_CMP - 1)))
        p_chunk = sum(p_grp[..., R_CMP - 1 - n:R_CMP - 1 - n + n_chunk] for n in range(R_CMP))
        p_slc = p_chunk.reshape(b_, N_KV, NSA_QB, n_slc, R_SLC).sum(axis=-1)
        cur = t // L_SLC
        valid = blk_ids[None, :] <= cur[:, None]
        forced = valid & ((blk_ids[None, :] == 0) | (blk_ids[None, :] > cur[:, None] - N_LOCAL))
        score = jnp.where(forced, jnp.inf, jnp.where(valid, p_slc, -jnp.inf))
        _, sel = lax.top_k(score, k_top)
        ks_sel = gather_blocks(ks_blocks, sel)
        vs_sel = gather_blocks(vs_blocks, sel)
        pos_s = sel[..., None] * L_SLC + jnp.arange(L_SLC)
        dist_s = t[:, None, None] - pos_s
        mask_s = (sel <= cur[:, None])[..., None] & (dist_s >= 0)
        s_s = jnp.einsum('bghqd,bgqkld->bghqkl', qb, ks_sel).astype(jnp.float32) * scale - slopes[..., None] * dist_s[:, :, None].astype(jnp.float32)
        n_keys = k_top * L_SLC
        p_s = masked_softmax(s_s.reshape(b_, N_KV, HEADS_PER_KV, NSA_QB, n_keys), mask_s.reshape(b_, N_KV, 1, NSA_QB, n_keys))
        o_s = jnp.einsum('bghqn,bgqnd->bghqd', p_s.astype(vs_sel.dtype), vs_sel.reshape(b_, N_KV, NSA_QB, n_keys, HEAD_DIM))
        kwin = lax.dynamic_slice_in_dim(kw_pad, s0, W_WIN + NSA_QB, axis=2)
        vwin = lax.dynamic_slice_in_dim(vw_pad, s0, W_WIN + NSA_QB, axis=2)
        pos_w = s0 - W_WIN + win_offsets
        dist_w = t[:, None] - pos_w[None, :]
        mask_w = (pos_w[None, :] >= 0) & (dist_w >= 0) & (dist_w < W_WIN)
        s_w = jnp.einsum('bghqd,bgkd->bghqk', qb, kwin).astype(jnp.float32) * scale - slopes * dist_w.astype(jnp.float32)
        p_w = masked_softmax(s_w, mask_w)
        o_w = jnp.einsum('bghqk,bgkd->bghqd', p_w.astype(vwin.dtype), vwin)
        out = gb[..., 0:1] * o_c + gb[..., 1:2] * o_s + gb[..., 2:3] * o_w
        return out.astype(qb.dtype)

    out = lax.map(block_fn, (q_blocks, g_blocks, jnp.arange(n_qb)))
    out = jnp.moveaxis(out, 0, 3).reshape(b_, N_KV, HEADS_PER_KV, t_, HEAD_DIM)
    return out.transpose(0, 3, 1, 2, 4).reshape(b_, t_, D_ATTN)


def rglru_mixer(xr, yg, conv_w, conv_b, wa, ba, wx, bx, lam):
    b_, t_, c_ = xr.shape
    xc = lax.conv_general_dilated(xr, conv_w[:, None, :], window_strides=(1,), padding=[(CONV_W - 1, 0)],
                                  dimension_numbers=('NWC', 'WIO', 'NWC'), feature_group_count=c_) + conv_b
    xb = xc.reshape(b_, t_, N_REC_BLOCKS, REC_BW)
    r = jax.nn.sigmoid((jnp.einsum('btnc,ncd->btnd', xb, wa).reshape(b_, t_, c_) + ba).astype(jnp.float32))
    i = jax.nn.sigmoid((jnp.einsum('btnc,ncd->btnd', xb, wx).reshape(b_, t_, c_) + bx).astype(jnp.float32))
    log_a = -C_LRU * r * jax.nn.softplus(-lam.astype(jnp.float32))
    a = jnp.exp(log_a)
    b = jnp.sqrt(-jnp.expm1(2.0 * log_a)) * i * xc.astype(jnp.float32)

    def combine(left, right):
        a_l, b_l = left
        a_r, b_r = right
        return a_l * a_r, a_r * b_l + b_r

    _, h = lax.associative_scan(combine, (a, b), axis=1)
    return (h * jax.nn.gelu(yg.astype(jnp.float32))).astype(xr.dtype)


def dense_swiglu(h, wg, wu, wd):
    return (jax.nn.silu(h @ wg) * (h @ wu)) @ wd


def moe_swiglu(h, router_w, wg, wu, wd):
    n_tok, d_ = h.shape
    nk = n_tok * TOP_K
    n_blocks = -(-nk // MOE_BLOCK) + N_EXPERTS
    n_slots = n_blocks * MOE_BLOCK
    logits = (h @ router_w).astype(jnp.float32)
    top_logit, top_e = lax.top_k(logits, TOP_K)
    gates = jax.nn.softmax(top_logit, axis=-1).astype(h.dtype)
    flat_e = top_e.reshape(-1)
    flat_tok = jnp.arange(nk, dtype=jnp.int32) // TOP_K
    flat_g = gates.reshape(-1)
    order = jnp.argsort(flat_e, stable=True)
    e_sorted = flat_e[order]
    counts = jnp.bincount(flat_e, length=N_EXPERTS)
    starts = jnp.cumsum(counts) - counts
    padded = (counts + MOE_BLOCK - 1) // MOE_BLOCK * MOE_BLOCK
    pad_ends = jnp.cumsum(padded)
    pad_starts = pad_ends - padded
    dest = pad_starts[e_sorted] + jnp.arange(nk) - starts[e_sorted]
    slot_tok = jnp.full((n_slots,), n_tok, jnp.int32).at[dest].set(flat_tok[order])
    slot_g = jnp.zeros((n_slots,), h.dtype).at[dest].set(flat_g[order])
    block_e = jnp.minimum(jnp.searchsorted(pad_ends, jnp.arange(n_blocks) * MOE_BLOCK, side='right'), N_EXPERTS - 1)
    h_pad = jnp.concatenate([h, jnp.zeros((1, d_), h.dtype)], axis=0)
    xb = h_pad[slot_tok].reshape(n_blocks, MOE_BLOCK, d_)

    def expert_block(args):
        xblk, e = args
        return (jax.nn.silu(xblk @ wg[e]) * (xblk @ wu[e])) @ wd[e]

    yb = lax.map(expert_block, (xb, block_e)).reshape(n_slots, d_)
    y = jax.ops.segment_sum(yb * slot_g[:, None], slot_tok, num_segments=n_tok + 1)
    return y[:n_tok]


def setup_inputs(seed: int = 0) -> dict:
    key = jax.random.key(seed)
    k = jax.random.split(key, 32)
    f32 = jnp.float32
    nrm = lambda kk, shape, fan_in: jax.random.normal(kk, shape, f32) * (fan_in ** -0.5)
    gain = lambda kk, shape: 1.0 + 0.05 * jax.random.normal(kk, shape, f32)
    small = lambda kk, shape: 0.01 * jax.random.normal(kk, shape, f32)
    a0 = jax.random.uniform(k[13], (DEPTH, D_REC), f32, 0.9, 0.999)
    sig = a0 ** (1.0 / C_LRU)
    lru_lambda = jnp.log(sig) - jnp.log1p(-sig)
    return {
        'x': jax.random.normal(k[0], (BATCH, SEQ, D_MODEL), f32),
        'p': jax.random.normal(k[1], (DEPTH, BATCH, SEQ, PLE_DIM), f32),
        'attn_norm': gain(k[2], (DEPTH, D_MODEL)),
        'w_in': nrm(k[3], (DEPTH, D_MODEL, IN_COLS), D_MODEL),
        'cmp_pos': 0.02 * jax.random.normal(k[4], (DEPTH, 2, L_CMP, HEAD_DIM), f32),
        'cmp_w1': nrm(k[5], (DEPTH, 2, L_CMP * HEAD_DIM, CMP_HID), L_CMP * HEAD_DIM),
        'cmp_w2': nrm(k[6], (DEPTH, 2, CMP_HID, HEAD_DIM), CMP_HID),
        'conv_w': nrm(k[7], (DEPTH, CONV_W, D_REC), CONV_W),
        'conv_b': small(k[8], (DEPTH, D_REC)),
        'lru_wa': nrm(k[9], (DEPTH, N_REC_BLOCKS, REC_BW, REC_BW), REC_BW),
        'lru_ba': small(k[10], (DEPTH, D_REC)),
        'lru_wx': nrm(k[11], (DEPTH, N_REC_BLOCKS, REC_BW, REC_BW), REC_BW),
        'lru_bx': small(k[12], (DEPTH, D_REC)),
        'lru_lambda': lru_lambda,
        'out_norm_attn': gain(k[14], (DEPTH, D_ATTN)),
        'out_norm_rec': gain(k[15], (DEPTH, D_REC)),
        'w_out': nrm(k[16], (DEPTH, D_MODEL, D_MODEL), D_MODEL),
        'ffn_norm': gain(k[17], (DEPTH, D_MODEL)),
        'dense_w_gate': nrm(k[18], (N_DENSE, D_MODEL, D_FF), D_MODEL),
        'dense_w_up': nrm(k[19], (N_DENSE, D_MODEL, D_FF), D_MODEL),
        'dense_w_down': nrm(k[20], (N_DENSE, D_FF, D_MODEL), D_FF),
        'router_w': nrm(k[21], (N_MOE, D_MODEL, N_EXPERTS), D_MODEL),
        'moe_w_gate': nrm(k[22], (N_MOE, N_EXPERTS, D_MODEL, D_FF_EXPERT), D_MODEL),
        'moe_w_up': nrm(k[23], (N_MOE, N_EXPERTS, D_MODEL, D_FF_EXPERT), D_MODEL),
        'moe_w_down': nrm(k[24], (N_MOE, N_EXPERTS, D_FF_EXPERT, D_MODEL), D_FF_EXPERT),
        'ple_norm': gain(k[25], (DEPTH, D_MODEL)),
        'ple_w_gate': nrm(k[26], (DEPTH, D_MODEL, D_MODEL), D_MODEL),
        'ple_w_proj': nrm(k[27], (DEPTH, PLE_DIM, D_MODEL), PLE_DIM),
        'final_norm': gain(k[28], (D_MODEL,)),
    }


def reference(x, p, attn_norm, w_in, cmp_pos, cmp_w1, cmp_w2, conv_w, conv_b, lru_wa, lru_ba, lru_wx, lru_bx,
              lru_lambda, out_norm_attn, out_norm_rec, w_out, ffn_norm, dense_w_gate, dense_w_up, dense_w_down,
              router_w, moe_w_gate, moe_w_up, moe_w_down, ple_norm, ple_w_gate, ple_w_proj, final_norm):
    b_, t_, _ = x.shape
    split_idx = np.cumsum(IN_SPLITS)[:-1].tolist()
    for i in range(DEPTH):
        h = rmsnorm(x, attn_norm[i])
        q, kc, vc, ks_, vs_, kw, vw, g, xr, yg = jnp.split(h @ w_in[i], split_idx, axis=-1)
        kvh = (b_, t_, N_KV, HEAD_DIM)
        o_attn = nsa_attention(q.reshape(b_, t_, N_ATTN_HEADS, HEAD_DIM), kc.reshape(kvh), vc.reshape(kvh),
                               ks_.reshape(kvh), vs_.reshape(kvh), kw.reshape(kvh), vw.reshape(kvh),
                               g.reshape(b_, t_, N_ATTN_HEADS, N_GATES), cmp_pos[i], cmp_w1[i], cmp_w2[i])
        o_rec = rglru_mixer(xr, yg, conv_w[i], conv_b[i], lru_wa[i], lru_ba[i], lru_wx[i], lru_bx[i], lru_lambda[i])
        mixed = jnp.concatenate([rmsnorm(o_attn, out_norm_attn[i]), rmsnorm(o_rec, out_norm_rec[i])], axis=-1)
        x = x + mixed @ w_out[i]
        h = rmsnorm(x, ffn_norm[i])
        j = i // 2
        if i % 2 == 0:
            y = dense_swiglu(h, dense_w_gate[j], dense_w_up[j], dense_w_down[j])
        else:
            y = moe_swiglu(h.reshape(b_ * t_, D_MODEL), router_w[j], moe_w_gate[j], moe_w_up[j],
                           moe_w_down[j]).reshape(b_, t_, D_MODEL)
        x = x + y
        gate = jax.nn.sigmoid(rmsnorm(x, ple_norm[i]) @ ple_w_gate[i])
        x = x + gate * (p[i] @ ple_w_proj[i])
    return rmsnorm(x, final_norm)
```

```python
import numpy as np
from contextlib import ExitStack
import concourse.bass as bass
import concourse.mybir as mybir
from concourse.bass_utils import run_bass_kernel_spmd

F32 = mybir.dt.float32
BF16 = mybir.dt.bfloat16
ALU = mybir.AluOpType
AF = mybir.ActivationFunctionType
AX = mybir.AxisListType
ENGS = ["sync", "act", "pe", "dve", "pool"]


class Buf:
    def __init__(self, name, t):
        self.name = name
        self.t = t
        self.last_w = None
        self.readers = []

    def __getitem__(self, idx):
        return self.t[idx]


class Prog:
    def __init__(self, same_engine_sync=True):
        self.nc = bass.Bass("TRN2", target_bir_lowering=False)
        self.es = ExitStack()
        self.ops = {e: [] for e in ENGS}
        self.cnt = {}
        self.sems = {}
        self.waited = {e: {} for e in ENGS}
        self.same = same_engine_sync
        self.nbuf = 0

    def sbuf(self, name, shape, dt):
        self.nbuf += 1
        name = f"{name}_{self.nbuf}"
        t = self.es.enter_context(self.nc.sbuf_tensor(name, list(shape), dt))
        return Buf(name, t)

    def psum(self, name, shape, dt=F32):
        self.nbuf += 1
        name = f"{name}_{self.nbuf}"
        t = self.es.enter_context(self.nc.psum_tensor(name, list(shape), dt))
        return Buf(name, t)

    def dram(self, name, shape, dt, kind):
        t = self.nc.dram_tensor(name, list(shape), dt, kind=kind)
        return Buf(name, t.ap())

    def _sem(self, key):
        if key not in self.sems:
            nm = "s_" + "_".join(str(k) for k in key)
            self.sems[key] = self.es.enter_context(self.nc.semaphore(nm))
            self.cnt[key] = 0
        return self.sems[key]

    def op(self, eng, fn, reads=(), writes=(), dma=None):
        deps = {}

        def add(tok):
            k, v = tok
            if k[0] == "D":
                v = self.cnt[k]
            if deps.get(k, 0) < v:
                deps[k] = v

        for b in reads:
            if b.last_w is not None:
                add(b.last_w)
        for b in writes:
            if b.last_w is not None:
                add(b.last_w)
            for r in b.readers:
                add(r)
        if dma is not None:
            key = ("D", dma.name)
            self._sem(key)
            self.cnt[key] += 16
            inc = 16
        else:
            key = ("E", eng)
            self._sem(key)
            self.cnt[key] += 1
            inc = 1
        tok = (key, self.cnt[key])
        waits = []
        for k, v in deps.items():
            if k == ("E", eng) and (eng == "pe" or not self.same):
                continue
            if self.waited[eng].get(k, 0) >= v:
                continue
            self.waited[eng][k] = v
            waits.append((k, v))
        self.ops[eng].append((waits, fn, key, inc))
        for b in reads:
            b.readers.append(tok)
        for b in writes:
            b.last_w = tok
            b.readers = []
        return tok

    def I(self, eng, method, *args, reads=(), writes=(), **kw):
        return self.op(eng, lambda e: getattr(e, method)(*args, **kw), reads=reads, writes=writes)

    def dma(self, q, out_ap, in_ap, reads=(), writes=(), track=None, **kw):
        return self.op(q, lambda e: e.dma_start(out=out_ap, in_=in_ap, **kw),
                       reads=reads, writes=writes, dma=track)

    def finish(self):
        waits = []
        for k, v in self.cnt.items():
            if k[0] == "D" and v > 0:
                waits.append((k, v))
        self.ops["sync"].append((waits, None, None, 0))
        nc = self.nc
        prog = self

        def emit(e, name):
            for waits, fn, key, inc in prog.ops[name]:
                for k, v in waits:
                    e.wait_ge(prog.sems[k], v)
                if fn is not None:
                    ins = fn(e)
                    ins.then_inc(prog.sems[key], inc)

        with nc.Block() as block:
            @block.sync
            def _(e):
                emit(e, "sync")

            @block.scalar
            def _(e):
                emit(e, "act")

            @block.tensor
            def _(e):
                emit(e, "pe")

            @block.vector
            def _(e):
                emit(e, "dve")

            @block.gpsimd
            def _(e):
                emit(e, "pool")
        self.es.close()
        return nc

    def n_ops(self):
        return {e: len(v) for e, v in self.ops.items()}


NCORES = 8
TOK = 4096
SEQ = 8192
D = 1024
INC = 2328
EPS = 1e-6
NEG = -30000.0
GELU_C = 1.5957691216057308


def run_spmd(nc, in_maps):
    res = run_bass_kernel_spmd(nc, in_maps, core_ids=list(range(len(in_maps))))
    return res.results


def emit_norm(P, sq, ss, rstd, col, src_ap, width, gain_ap, dst_ap, src_bufs, gain_buf, dst_buf, extra32=None):
    P.I("act", "activation", out=sq[:, 0:width], in_=src_ap, func=AF.Square, accum_out=ss[:, col:col + 1],
        reads=src_bufs, writes=[sq, ss])
    P.I("dve", "tensor_scalar", out=rstd[:, col:col + 1], in0=ss[:, col:col + 1], scalar1=1.0 / width, scalar2=EPS,
        op0=ALU.mult, op1=ALU.add, reads=[ss], writes=[rstd])
    P.I("act", "sqrt", out=rstd[:, col:col + 1], in_=rstd[:, col:col + 1], reads=[rstd], writes=[rstd])
    P.I("dve", "reciprocal", out=rstd[:, col:col + 1], in_=rstd[:, col:col + 1], reads=[rstd], writes=[rstd])
    if extra32 is not None:
        P.I("dve", "scalar_tensor_tensor", out=extra32[:, :], in0=src_ap, scalar=rstd[:, col:col + 1], in1=gain_ap,
            op0=ALU.mult, op1=ALU.mult, reads=list(src_bufs) + [rstd, gain_buf], writes=[extra32])
        P.I("pool", "tensor_copy", out=dst_ap, in_=extra32[:, :], reads=[extra32], writes=[dst_buf])
    else:
        P.I("dve", "scalar_tensor_tensor", out=dst_ap, in0=src_ap, scalar=rstd[:, col:col + 1], in1=gain_ap,
            op0=ALU.mult, op1=ALU.mult, reads=list(src_bufs) + [rstd, gain_buf], writes=[dst_buf])


def emit_transpose8(P, tp, id_bf, h_, dst_ap, dst_buf):
    for kc in range(8):
        P.I("pe", "transpose", out=tp[:, kc * 128:(kc + 1) * 128], in_=h_[:, kc * 128:(kc + 1) * 128],
            identity=id_bf[:, :], reads=[h_, id_bf], writes=[tp])
    P.I("act", "copy", out=dst_ap, in_=tp[:, :].rearrange("p (a b) -> p a b", a=8), reads=[tp], writes=[dst_buf])


def build_l1(tok=TOK):
    P = Prog()
    x = P.dram("x", [tok, D], F32, "ExternalInput")
    w = P.dram("w", [D, INC], F32, "ExternalInput")
    g = P.dram("g", [1, D], F32, "ExternalInput")
    ident = P.dram("ident", [128, 128], F32, "ExternalInput")
    out = P.dram("proj", [tok, INC], F32, "ExternalOutput")
    w_sb = P.sbuf("w", [128, 8, INC], BF16)
    g_bc = P.sbuf("g", [128, D], F32)
    id_bf = P.sbuf("id", [128, 128], BF16)
    for kc in range(8):
        P.dma("pool", w_sb[:, kc, :], w[kc * 128:(kc + 1) * 128, :], writes=[w_sb], track=w_sb)
    P.dma("sync", g_bc[:, :], g[0:1, :].partition_broadcast(128), writes=[g_bc], track=g_bc)
    P.dma("pool", id_bf[:, :], ident[:, :], writes=[id_bf], track=id_bf)
    xin = [P.sbuf("xin", [128, D], F32) for _ in range(2)]
    sq = P.sbuf("sq", [128, D], BF16)
    ss = P.sbuf("ss", [128, 2], F32)
    rstd = P.sbuf("rstd", [128, 2], F32)
    hn = [P.sbuf("hn", [128, D], BF16) for _ in range(2)]
    tp = P.psum("tp", [128, D], BF16)
    hT = [P.sbuf("hT", [128, 8, 128], BF16) for _ in range(2)]
    acc = [P.psum("acc", [128, 512], F32) for _ in range(4)]
    ot = [P.sbuf("ot", [128, INC], F32) for _ in range(2)]
    chunks = [(c0, min(512, INC - c0)) for c0 in range(0, INC, 512)]
    nacc = 0
    nt = tok // 128
    P.dma("sync", xin[0][:, :], x[0:128, :], writes=[xin[0]], track=xin[0])
    for t in range(nt):
        b = t % 2
        xi, h_, hT_, ot_ = xin[b], hn[b], hT[b], ot[b]
        if t + 1 < nt:
            xn = xin[(t + 1) % 2]
            P.dma("sync", xn[:, :], x[(t + 1) * 128:(t + 2) * 128, :], writes=[xn], track=xn)
        emit_norm(P, sq, ss, rstd, b, xi[:, :], D, g_bc[:, :], h_[:, :], [xi], g_bc, h_)
        emit_transpose8(P, tp, id_bf, h_, hT_[:, :, :], hT_)
        for ci, (c0, n) in enumerate(chunks):
            a_ = acc[nacc % 4]
            nacc += 1
            for kc in range(8):
                P.I("pe", "matmul", a_[:, 0:n], lhsT=hT_[:, kc, :], rhs=w_sb[:, kc, c0:c0 + n],
                    start=(kc == 0), stop=(kc == 7), reads=[hT_, w_sb], writes=[a_])
            if ci % 2 == 0:
                P.I("dve", "tensor_copy", out=ot_[:, c0:c0 + n], in_=a_[:, 0:n], reads=[a_], writes=[ot_])
            else:
                P.I("act", "copy", out=ot_[:, c0:c0 + n], in_=a_[:, 0:n], reads=[a_], writes=[ot_])
        P.dma("sync", out[t * 128:(t + 1) * 128, :], ot_[:, :], reads=[ot_], track=ot_)
    return P.finish()


def build_l2a(seq=SEQ):
    P = Prog()
    NQ = seq // 128
    NCT = seq // 2048
    qa = P.dram("qa", [68, 4, seq], F32, "ExternalInput")
    kasd = P.dram("kas", [68, seq], F32, "ExternalInput")
    kawd = P.dram("kaw", [68, seq], F32, "ExternalInput")
    vasd = P.dram("vas", [seq, 65], F32, "ExternalInput")
    vawd = P.dram("vaw", [seq, 65], F32, "ExternalInput")
    kvTd = [P.dram("kcT", [64, seq], F32, "ExternalInput"), P.dram("vcT", [64, seq], F32, "ExternalInput")]
    w1d = [P.dram("w1k", [64, 32, 128], F32, "ExternalInput"), P.dram("w1v", [64, 32, 128], F32, "ExternalInput")]
    w2d = [P.dram("w2k", [128, 64], F32, "ExternalInput"), P.dram("w2v", [128, 64], F32, "ExternalInput")]
    posd = [P.dram("posk", [64, 32], F32, "ExternalInput"), P.dram("posv", [64, 32], F32, "ExternalInput")]
    kcc = P.dram("kcc", [4, NCT * 128], F32, "ExternalInput")
    vones = P.dram("vones", [128, NCT], F32, "ExternalInput")
    aaug = P.dram("aaug", [128, NCT, 129], F32, "ExternalInput")
    gatesd = P.dram("gates", [seq, 12], F32, "ExternalInput")
    ident = P.dram("ident", [128, 128], F32, "ExternalInput")
    eall = P.dram("eall", [128, seq], F32, "ExternalInput")
    trid = P.dram("tri", [128, 128], F32, "ExternalInput")
    woldd = P.dram("wold", [128, 128], F32, "ExternalInput")
    cmd = P.dram("cm", [128, 17, 128], F32, "ExternalInput")
    ftabd = P.dram("ftab", [128, 256], F32, "ExternalInput")
    oat = P.dram("oat", [seq, 256], F32, "ExternalOutput")

    kas = P.sbuf("kas", [68, seq], BF16)
    kaw = P.sbuf("kaw", [68, seq], BF16)
    vas = P.sbuf("vas", [128, NQ, 65], BF16)
    vaw = P.sbuf("vaw", [128, NQ, 65], BF16)
    E = P.sbuf("E", [128, seq], BF16)
    cm4 = P.sbuf("cm4", [128, 17, 512], BF16)
    tri4 = P.sbuf("tri4", [128, 512], BF16)
    wold4 = P.sbuf("wold4", [128, 512], BF16)
    A = P.sbuf("A", [128, NCT, 129], BF16)
    id_bf = P.sbuf("idbf", [128, 128], BF16)
    id32 = P.sbuf("id32", [128, 128], F32)
    ftab = P.sbuf("ftab", [128, 256], F32)
    kca = P.sbuf("kca", [68, NCT * 128], BF16)
    vca = P.sbuf("vca", [128, NCT, 65], BF16)
    ones_sb = P.sbuf("ones", [128, NCT], F32)
    P.dma("pool", kas[:, :], kasd[:, :], writes=[kas], track=kas)
    P.dma("pool", kaw[:, :], kawd[:, :], writes=[kaw], track=kaw)
    P.dma("pool", vas[:, :, :], vasd[:, :].rearrange("(j p) d -> p j d", p=128), writes=[vas], track=vas)
    P.dma("pool", vaw[:, :, :], vawd[:, :].rearrange("(j p) d -> p j d", p=128), writes=[vaw], track=vaw)
    P.dma("pool", E[:, :], eall[:, :], writes=[E], track=E)
    for h in range(4):
        P.dma("pool", cm4[:, :, h * 128:(h + 1) * 128], cmd[:, :, :], writes=[cm4], track=cm4)
        P.dma("pool", tri4[:, h * 128:(h + 1) * 128], trid[:, :], writes=[tri4], track=tri4)
        P.dma("pool", wold4[:, h * 128:(h + 1) * 128], woldd[:, :], writes=[wold4], track=wold4)
    P.dma("pool", A[:, :, :], aaug[:, :, :], writes=[A], track=A)
    P.dma("pool", id_bf[:, :], ident[:, :], writes=[id_bf], track=id_bf)
    P.dma("sync", id32[:, :], ident[:, :], writes=[id32], track=id32)
    P.dma("sync", ftab[:, :], ftabd[:, :], writes=[ftab], track=ftab)
    P.dma("sync", ones_sb[:, :], vones[:, :], writes=[ones_sb], track=ones_sb)

    sps = [P.psum("sps", [128, 512], F32) for _ in range(2)]
    oac = [P.psum("oac", [128, 512], F32) for _ in range(3)]
    miscA = P.psum("miscA", [128, 512], F32)
    miscB = P.psum("miscB", [128, 512], F32)
    nmT = P.psum("nmT", [128, 128], BF16)

    P.I("dve", "memset", kca[:, :], 0.0, writes=[kca])
    P.I("dve", "memset", vca[:, :, :], 0.0, writes=[vca])
    P.dma("pool", kca[64:68, :], kcc[:, :], reads=[], writes=[kca], track=kca)
    w1 = [P.sbuf("w1", [64, 32, 128], BF16) for _ in range(2)]
    w2 = [P.sbuf("w2", [128, 64], BF16) for _ in range(2)]
    posT = [P.sbuf("posT", [64, 32], BF16) for _ in range(2)]
    biasv = P.sbuf("biasv", [128, 2], F32)
    kvch = [P.sbuf("kvch", [64, 2064], BF16) for _ in range(2)]
    hx = P.sbuf("hx", [128, 128], F32)
    hu = P.sbuf("hu", [128, 128], F32)
    G = P.sbuf("G", [128, 128], BF16)
    nk = 0
    for kv in range(2):
        P.dma("pool", w1[kv][:, :, :], w1d[kv][:, :, :], writes=[w1[kv]], track=w1[kv])
        P.dma("pool", w2[kv][:, :], w2d[kv][:, :], writes=[w2[kv]], track=w2[kv])
        P.dma("pool", posT[kv][:, :], posd[kv][:, :], writes=[posT[kv]], track=posT[kv])
        for j in range(32):
            P.I("pe", "matmul", miscB[:, 0:1], lhsT=w1[kv][:, j, :], rhs=posT[kv][:, j:j + 1], start=(j == 0),
                stop=(j == 31), reads=[w1[kv], posT[kv]], writes=[miscB])
        P.I("dve", "tensor_copy", out=biasv[:, kv:kv + 1], in_=miscB[:, 0:1], reads=[miscB], writes=[biasv])
        for jc in range(NCT):
            n_c = 128 if jc < NCT - 1 else 127
            L = 16 * (n_c - 1) + 32
            ch = kvch[nk % 2]
            nk += 1
            P.dma("pool", ch[:, 0:L], kvTd[kv][:, 2048 * jc:2048 * jc + L], writes=[ch], track=ch)
            for j in range(32):
                P.I("pe", "matmul", sps[0][:, 0:n_c], lhsT=w1[kv][:, j, :], rhs=ch[:, j:j + 16 * (n_c - 1) + 1:16],
                    start=(j == 0), stop=(j == 31), reads=[w1[kv], ch], writes=[sps[0]])
            P.I("act", "activation", out=hx[:, 0:n_c], in_=sps[0][:, 0:n_c], func=AF.Identity, bias=biasv[:, kv:kv + 1],
                reads=[sps[0], biasv], writes=[hx])
            P.I("dve", "tensor_tensor", out=hu[:, 0:n_c], in0=hx[:, 0:n_c], in1=hx[:, 0:n_c], op=ALU.mult,
                reads=[hx], writes=[hu])
            P.I("dve", "tensor_scalar", out=hu[:, 0:n_c], in0=hu[:, 0:n_c], scalar1=0.044715, scalar2=1.0,
                op0=ALU.mult, op1=ALU.add, reads=[hu], writes=[hu])
            P.I("dve", "tensor_tensor", out=hu[:, 0:n_c], in0=hu[:, 0:n_c], in1=hx[:, 0:n_c], op=ALU.mult,
                reads=[hu, hx], writes=[hu])
            P.I("act", "activation", out=hu[:, 0:n_c], in_=hu[:, 0:n_c], func=AF.Sigmoid, scale=GELU_C,
                reads=[hu], writes=[hu])
            P.I("dve", "tensor_tensor", out=G[:, 0:n_c], in0=hu[:, 0:n_c], in1=hx[:, 0:n_c], op=ALU.mult,
                reads=[hu, hx], writes=[G])
            if kv == 0:
                P.I("pe", "matmul", sps[1][0:64, 0:n_c], lhsT=w2[0][:, :], rhs=G[:, 0:n_c], start=True, stop=True,
                    reads=[w2[0], G], writes=[sps[1]])
                P.I("act", "copy", out=kca[0:64, jc * 128:jc * 128 + n_c], in_=sps[1][0:64, 0:n_c],
                    reads=[sps[1]], writes=[kca])
            else:
                P.I("pe", "matmul", miscA[0:n_c, 0:64], lhsT=G[:, 0:n_c], rhs=w2[1][:, :], start=True, stop=True,
                    reads=[w2[1], G], writes=[miscA])
                P.I("dve", "tensor_copy", out=vca[0:n_c, jc, 0:64], in_=miscA[0:n_c, 0:64], reads=[miscA], writes=[vca])
    P.I("dve", "tensor_copy", out=vca[:, :, 64], in_=ones_sb[:, :], reads=[ones_sb], writes=[vca])

    qt = [P.sbuf("qt", [68, 4, 128], BF16) for _ in range(2)]
    gt = [P.sbuf("gt", [128, 12], F32) for _ in range(2)]
    sgt = P.sbuf("sgt", [128, 12], F32)
    Pc = P.sbuf("Pc", [128, NCT, 512], BF16)
    Ps = [P.sbuf("Ps", [128, 512], BF16) for _ in range(3)]
    nm4 = P.sbuf("nm4", [128, 512], BF16)
    nmq = P.sbuf("nmq", [128, 128], BF16)
    osb = [P.sbuf("osb", [65, 512], F32) for _ in range(3)]
    score = P.sbuf("score", [128, 128], F32)
    sc2 = P.sbuf("sc2", [128, 128], F32)
    m8a = P.sbuf("m8a", [128, 8], F32)
    m8b = P.sbuf("m8b", [128, 8], F32)
    rzc = P.sbuf("rzc", [128, 4], F32)
    rz = P.sbuf("rz", [128, 4], F32)
    coef = P.sbuf("coef", [128, 4], F32)
    oacc = [P.sbuf("oacc", [128, 256], F32) for _ in range(2)]
    nps = 0

    def load_q(i):
        q_, g_ = qt[i % 2], gt[i % 2]
        P.dma("pool", q_[:, :, :], qa[:, :, i * 128:(i + 1) * 128], writes=[q_], track=q_)
        P.dma("sync", g_[:, :], gatesd[i * 128:(i + 1) * 128, :], writes=[g_], track=g_)

    load_q(0)
    nsp = 0
    for i in range(NQ):
        q_, g_ = qt[i % 2], gt[i % 2]
        if i + 1 < NQ:
            load_q(i + 1)
        Q = q_[:, :, :].rearrange("p h q -> p (h q)")
        P.I("act", "activation", out=sgt[:, :], in_=g_[:, :], func=AF.Sigmoid, reads=[g_], writes=[sgt])
        tasks = []
        for jc in range(NCT):
            dlt = i - 16 * jc
            if dlt < 0:
                continue
            masks = [(id_bf[:, :], cm4[:, dlt, :], [id_bf, cm4])] if dlt <= 16 else []
            tasks.append((0, kca[:, jc * 128:(jc + 1) * 128], kca, masks, vca[:, jc, :], vca, Pc[:, jc, :], Pc))
        n_cmp = len(tasks)
        for j in range(max(0, i - 4), i + 1):
            masks = []
            if j == i - 4:
                masks.append((id_bf[:, :], wold4[:, :], [id_bf, wold4]))
            if j == i:
                masks.append((id_bf[:, :], tri4[:, :], [id_bf, tri4]))
            tasks.append((2, kaw[:, j * 128:(j + 1) * 128], kaw, masks, vaw[:, j, :], vaw, None, None))
        for j in range(0, i + 1):
            masks = [(E[:, j * 128:(j + 1) * 128], nm4[:, :], [E, nm4])]
            if j == i:
                masks.append((id_bf[:, :], tri4[:, :], [id_bf, tri4]))
            tasks.append((1, kas[:, j * 128:(j + 1) * 128], kas, masks, vas[:, j, :], vas, None, None))
        first = {}
        last = {}
        for ti, t_ in enumerate(tasks):
            first.setdefault(t_[0], ti)
            last[t_[0]] = ti

        pend = None

        def emit_pv(pd):
            br, ti, vap, vbuf, pap, pbuf = pd
            P.I("pe", "matmul", oac[br][0:65, :], lhsT=vap, rhs=pap, start=(ti == first[br]), stop=(ti == last[br]),
                reads=[vbuf, pbuf], writes=[oac[br]])

        def emit_select():
            for h in range(4):
                dst = (miscA if h < 2 else miscB)
                c0 = (h % 2) * 129
                for jc in range(n_cmp):
                    P.I("pe", "matmul", dst[:, c0:c0 + 129], lhsT=Pc[:, jc, h * 128:(h + 1) * 128], rhs=A[:, jc, :],
                        start=(jc == 0), stop=(jc == n_cmp - 1), reads=[Pc, A], writes=[dst])
            for hp, mb in enumerate((miscA, miscB)):
                zv = mb[:, 0:258].rearrange("p (h d) -> p h d", d=129)[:, :, 128]
                P.I("dve", "tensor_scalar_max", out=rzc[:, 2 * hp:2 * hp + 2], in0=zv, scalar1=1e-30,
                    reads=[mb], writes=[rzc])
            P.I("dve", "reciprocal", out=rzc[:, :], in_=rzc[:, :], reads=[rzc], writes=[rzc])
            P.I("dve", "tensor_scalar_mul", out=score[:, :], in0=miscA[:, 0:128], scalar1=rzc[:, 0:1],
                reads=[miscA, rzc], writes=[score])
            for h in range(1, 4):
                mb = miscA if h < 2 else miscB
                c0 = (h % 2) * 129
                P.I("dve", "scalar_tensor_tensor", out=score[:, :], in0=mb[:, c0:c0 + 128], scalar=rzc[:, h:h + 1],
                    in1=score[:, :], op0=ALU.mult, op1=ALU.add, reads=[mb, rzc, score], writes=[score])
            P.I("dve", "tensor_tensor", out=score[:, :], in0=score[:, :], in1=ftab[:, 127 - 2 * i:255 - 2 * i],
                op=ALU.add, reads=[score, ftab], writes=[score])
            P.I("dve", "tensor_scalar_add", out=score[:, 0:1], in0=score[:, 0:1], scalar1=200.0,
                reads=[score], writes=[score])
            P.I("dve", "max", out=m8a[:, :], in_=score[:, :], reads=[score], writes=[m8a])
            P.I("dve", "match_replace", out=sc2[:, :], in_to_replace=m8a[:, :], in_values=score[:, :], imm_value=-1e9,
                reads=[m8a, score], writes=[sc2])
            P.I("dve", "max", out=m8b[:, :], in_=sc2[:, :], reads=[sc2], writes=[m8b])
            P.I("dve", "tensor_scalar", out=nmq[:, :], in0=score[:, :], scalar1=m8b[:, 7:8], scalar2=NEG,
                op0=ALU.is_lt, op1=ALU.mult, reads=[score, m8b], writes=[nmq])

        def emit_select2():
            P.I("pe", "transpose", out=nmT[:, :], in_=nmq[:, :], identity=id_bf[:, :], reads=[nmq, id_bf], writes=[nmT])
            P.I("dve", "tensor_copy", out=nm4[:, 0:128], in_=nmT[:, :], reads=[nmT], writes=[nm4])
            for h in range(1, 4):
                P.I("pool", "tensor_copy", out=nm4[:, h * 128:(h + 1) * 128], in_=nm4[:, 0:128], reads=[nm4], writes=[nm4])

        for ti, (br, lap, lbuf, masks, vap, vbuf, pap, pbuf) in enumerate(tasks):
            if ti == first[1]:
                emit_select2()
            sp = sps[nsp % 2]
            nsp += 1
            P.I("pe", "matmul", sp[:, :], lhsT=lap, rhs=Q, start=True, stop=(len(masks) == 0), reads=[lbuf, q_], writes=[sp])
            for mi, (ml, mr, mbufs) in enumerate(masks):
                P.I("pe", "matmul", sp[:, :], lhsT=ml, rhs=mr, start=False, stop=(mi == len(masks) - 1),
                    reads=mbufs, writes=[sp])
            if pend is not None:
                emit_pv(pend)
                pend = None
            if pap is None:
                pbuf = Ps[nps % 3]
                nps += 1
                pap = pbuf[:, :]
            P.I("act", "activation", out=pap, in_=sp[:, :], func=AF.Exp, scale=0.125, reads=[sp], writes=[pbuf])
            pend = (br, ti, vap, vbuf, pap, pbuf)
            if ti == n_cmp - 1:
                emit_pv(pend)
                pend = None
                emit_select()
        emit_pv(pend)
        o_ = oacc[i % 2]
        for br in range(3):
            P.I("dve", "tensor_copy", out=osb[br][:, :], in_=oac[br][0:65, :], reads=[oac[br]], writes=[osb[br]])
        for br in range(3):
            mb = miscA if br != 1 else miscB
            for h in range(4):
                P.I("pe", "transpose", out=mb[:, h * 65:(h + 1) * 65], in_=osb[br][:, h * 128:(h + 1) * 128],
                    identity=id32[0:65, 0:65], reads=[osb[br], id32], writes=[mb])
            mv = mb[:, 0:260].rearrange("p (h d) -> p h d", d=65)
            P.I("dve", "tensor_scalar_max", out=rz[:, :], in0=mv[:, :, 64], scalar1=1e-30, reads=[mb], writes=[rz])
            P.I("dve", "reciprocal", out=rz[:, :], in_=rz[:, :], reads=[rz], writes=[rz])
            gv = sgt[:, :].rearrange("p (h b) -> p h b", b=3)[:, :, br]
            P.I("dve", "tensor_tensor", out=coef[:, :], in0=rz[:, :], in1=gv, op=ALU.mult, reads=[rz, sgt], writes=[coef])
            for h in range(4):
                if br == 0:
                    P.I("dve", "tensor_scalar_mul", out=o_[:, h * 64:(h + 1) * 64], in0=mb[:, h * 65:h * 65 + 64],
                        scalar1=coef[:, h:h + 1], reads=[mb, coef], writes=[o_])
                else:
                    P.I("dve", "scalar_tensor_tensor", out=o_[:, h * 64:(h + 1) * 64], in0=mb[:, h * 65:h * 65 + 64],
                        scalar=coef[:, h:h + 1], in1=o_[:, h * 64:(h + 1) * 64], op0=ALU.mult, op1=ALU.add,
                        reads=[mb, coef, o_], writes=[o_])
        P.dma("sync", oat[i * 128:(i + 1) * 128, :], o_[:, :], reads=[o_], track=o_)
    return P.finish()


def build_l2b(seq=SEQ):
    P = Prog()
    CH = 1024
    NCH = seq // CH
    xrT = P.dram("xrT", [256, seq], F32, "ExternalInput")
    ygT = P.dram("ygT", [256, seq], F32, "ExternalInput")
    pvd = P.dram("pv", [128, 2, 8], F32, "ExternalInput")
    wabd = P.dram("wab", [128, 2, 128], F32, "ExternalInput")
    wxbd = P.dram("wxb", [128, 2, 128], F32, "ExternalInput")
    orT = P.dram("orT", [256, seq], F32, "ExternalOutput")

    pv = P.sbuf("pv", [128, 2, 8], F32)
    wab = P.sbuf("wab", [128, 2, 128], BF16)
    wxb = P.sbuf("wxb", [128, 2, 128], BF16)
    P.dma("sync", pv[:, :, :], pvd[:, :, :], writes=[pv], track=pv)
    P.dma("pool", wab[:, :, :], wabd[:, :, :], writes=[wab], track=wab)
    P.dma("pool", wxb[:, :, :], wxbd[:, :, :], writes=[wxb], track=wxb)
    sv = P.sbuf("sv", [128, 2, 8], F32)
    for ct in range(2):
        c = lambda k: sv[:, ct, k:k + 1]
        P.I("act", "activation", out=c(0), in_=pv[:, ct, 7:8], func=AF.Exp, scale=-1.0, reads=[pv], writes=[sv])
        P.I("act", "activation", out=c(1), in_=c(0), func=AF.Ln, bias=1.0, reads=[sv], writes=[sv])
        P.I("dve", "tensor_scalar", out=c(2), in0=c(0), scalar1=-0.25, scalar2=1.0 / 3.0, op0=ALU.mult, op1=ALU.add,
            reads=[sv], writes=[sv])
        P.I("dve", "tensor_tensor", out=c(2), in0=c(2), in1=c(0), op=ALU.mult, reads=[sv], writes=[sv])
        P.I("dve", "tensor_scalar_add", out=c(2), in0=c(2), scalar1=-0.5, reads=[sv], writes=[sv])
        P.I("dve", "tensor_tensor", out=c(2), in0=c(2), in1=c(0), op=ALU.mult, reads=[sv], writes=[sv])
        P.I("dve", "tensor_scalar_add", out=c(2), in0=c(2), scalar1=1.0, reads=[sv], writes=[sv])
        P.I("dve", "tensor_tensor", out=c(2), in0=c(2), in1=c(0), op=ALU.mult, reads=[sv], writes=[sv])
        P.I("dve", "tensor_single_scalar", out=c(3), in_=c(0), scalar=0.02, op=ALU.is_lt, reads=[sv], writes=[sv])
        P.I("dve", "tensor_tensor", out=c(4), in0=c(2), in1=c(1), op=ALU.subtract, reads=[sv], writes=[sv])
        P.I("dve", "tensor_tensor", out=c(4), in0=c(4), in1=c(3), op=ALU.mult, reads=[sv], writes=[sv])
        P.I("dve", "tensor_tensor", out=c(4), in0=c(4), in1=c(1), op=ALU.add, reads=[sv], writes=[sv])
        P.I("dve", "tensor_scalar_mul", out=c(5), in0=c(4), scalar1=-8.0, reads=[sv], writes=[sv])

    xrp = [P.sbuf("xrp", [128, CH + 3], F32) for _ in range(2)]
    ygb = [P.sbuf("ygb", [128, CH], F32) for _ in range(2)]
    xc = P.sbuf("xc", [128, CH], F32)
    xcb = P.sbuf("xcb", [128, CH], BF16)
    rr = P.sbuf("rr", [128, CH], F32)
    ii = P.sbuf("ii", [128, CH], F32)
    aa = P.sbuf("aa", [128, CH], F32)
    bb = P.sbuf("bb", [128, CH], F32)
    hh = [P.sbuf("hh", [128, CH], F32) for _ in range(2)]
    uu = P.sbuf("uu", [128, CH], F32)
    oo = [P.sbuf("oo", [128, CH], F32) for _ in range(2)]
    psr = [P.psum("psr", [128, 512], F32) for _ in range(2)]
    psi = [P.psum("psi", [128, 512], F32) for _ in range(2)]
    k = 0
    for ct in range(2):
        rows = slice(ct * 128, (ct + 1) * 128)
        for n in range(NCH):
            xp, yg, h_, o_ = xrp[k % 2], ygb[k % 2], hh[k % 2], oo[k % 2]
            hprev = hh[(k + 1) % 2]
            k += 1
            if n == 0:
                P.I("dve", "memset", xp[:, 0:3], 0.0, writes=[xp])
                P.dma("sync", xp[:, 3:3 + CH], xrT[rows, 0:CH], writes=[xp], track=xp)
            else:
                P.dma("sync", xp[:, 0:3 + CH], xrT[rows, n * CH - 3:(n + 1) * CH], writes=[xp], track=xp)
            P.dma("sync", yg[:, :], ygT[rows, n * CH:(n + 1) * CH], writes=[yg], track=yg)
            P.I("dve", "tensor_scalar", out=xc[:, :], in0=xp[:, 0:CH], scalar1=pv[:, ct, 0:1], scalar2=pv[:, ct, 4:5],
                op0=ALU.mult, op1=ALU.add, reads=[xp, pv], writes=[xc])
            for w_ in range(1, 4):
                P.I("dve", "scalar_tensor_tensor", out=xc[:, :], in0=xp[:, w_:w_ + CH], scalar=pv[:, ct, w_:w_ + 1],
                    in1=xc[:, :], op0=ALU.mult, op1=ALU.add, reads=[xp, pv, xc], writes=[xc])
            P.I("pool", "tensor_copy", out=xcb[:, :], in_=xc[:, :], reads=[xc], writes=[xcb])
            for hf in range(2):
                cs = slice(hf * 512, (hf + 1) * 512)
                P.I("pe", "matmul", psr[hf][:, :], lhsT=wab[:, ct, :], rhs=xcb[:, cs], start=True, stop=True,
                    reads=[wab, xcb], writes=[psr[hf]])
                P.I("pe", "matmul", psi[hf][:, :], lhsT=wxb[:, ct, :], rhs=xcb[:, cs], start=True, stop=True,
                    reads=[wxb, xcb], writes=[psi[hf]])
                P.I("act", "activation", out=rr[:, cs], in_=psr[hf][:, :], func=AF.Sigmoid, bias=pv[:, ct, 5:6],
                    reads=[psr[hf], pv], writes=[rr])
                P.I("act", "activation", out=ii[:, cs], in_=psi[hf][:, :], func=AF.Sigmoid, bias=pv[:, ct, 6:7],
                    reads=[psi[hf], pv], writes=[ii])
            P.I("act", "activation", out=aa[:, :], in_=rr[:, :], func=AF.Exp, scale=sv[:, ct, 5:6],
                reads=[rr, sv], writes=[aa])
            P.I("dve", "tensor_tensor", out=bb[:, :], in0=aa[:, :], in1=aa[:, :], op=ALU.mult, reads=[aa], writes=[bb])
            P.I("dve", "tensor_scalar", out=bb[:, :], in0=bb[:, :], scalar1=-1.0, scalar2=1.0, op0=ALU.mult, op1=ALU.add,
                reads=[bb], writes=[bb])
            P.I("act", "sqrt", out=bb[:, :], in_=bb[:, :], reads=[bb], writes=[bb])
            P.I("dve", "tensor_tensor", out=bb[:, :], in0=bb[:, :], in1=ii[:, :], op=ALU.mult, reads=[bb, ii], writes=[bb])
            P.I("dve", "tensor_tensor", out=bb[:, :], in0=bb[:, :], in1=xc[:, :], op=ALU.mult, reads=[bb, xc], writes=[bb])
            if n == 0:
                P.I("dve", "tensor_tensor_scan", out=h_[:, :], data0=aa[:, :], data1=bb[:, :], initial=0.0,
                    op0=ALU.mult, op1=ALU.add, reads=[aa, bb], writes=[h_])
            else:
                P.I("dve", "tensor_tensor_scan", out=h_[:, :], data0=aa[:, :], data1=bb[:, :],
                    initial=hprev[:, CH - 1:CH], op0=ALU.mult, op1=ALU.add, reads=[aa, bb, hprev], writes=[h_])
            P.I("pool", "tensor_tensor", out=uu[:, :], in0=yg[:, :], in1=yg[:, :], op=ALU.mult, reads=[yg], writes=[uu])
            P.I("pool", "tensor_scalar", out=uu[:, :], in0=uu[:, :], scalar1=0.044715, scalar2=1.0, op0=ALU.mult,
                op1=ALU.add, reads=[uu], writes=[uu])
            P.I("pool", "tensor_tensor", out=uu[:, :], in0=uu[:, :], in1=yg[:, :], op=ALU.mult, reads=[uu, yg], writes=[uu])
            P.I("act", "activation", out=uu[:, :], in_=uu[:, :], func=AF.Sigmoid, scale=GELU_C, reads=[uu], writes=[uu])
            P.I("pool", "tensor_tensor", out=uu[:, :], in0=uu[:, :], in1=yg[:, :], op=ALU.mult, reads=[uu, yg], writes=[uu])
            P.I("dve", "tensor_tensor", out=o_[:, :], in0=uu[:, :], in1=h_[:, :], op=ALU.mult, reads=[uu, h_], writes=[o_])
            P.dma("sync", orT[rows, n * CH:(n + 1) * CH], o_[:, :], reads=[o_], track=o_)
    return P.finish()


def build_l3(kind, final, tok=TOK):
    P = Prog()
    FF = 2816 if kind == "dense" else 3584
    NE = 1 if kind == "dense" else 8
    groups = [(f0, min(512, FF - f0)) for f0 in range(0, FF, 512)]
    x = P.dram("x", [tok, D], F32, "ExternalInput")
    oa = P.dram("oa", [tok, 512], F32, "ExternalInput")
    orc = P.dram("orc", [tok, 512], F32, "ExternalInput")
    pT = P.dram("pT", [256, tok], F32, "ExternalInput")
    gains = P.dram("gains", [4, D], F32, "ExternalInput")
    w_out = P.dram("w_out", [D, D], F32, "ExternalInput")
    ple_wg = P.dram("ple_wg", [D, D], F32, "ExternalInput")
    ple_wp = P.dram("ple_wp", [256, D], F32, "ExternalInput")
    ident = P.dram("ident", [128, 128], F32, "ExternalInput")
    wg = P.dram("wg", [NE, D, FF], F32, "ExternalInput")
    wu = P.dram("wu", [NE, D, FF], F32, "ExternalInput")
    wd = P.dram("wd", [NE, FF, D], F32, "ExternalInput")
    if kind == "moe":
        rwt = P.dram("rwt", [D, 8], F32, "ExternalInput")
    out = P.dram("out", [tok, D], F32, "ExternalOutput")

    id_bf = P.sbuf("id", [128, 128], BF16)
    P.dma("pool", id_bf[:, :], ident[:, :], writes=[id_bf], track=id_bf)
    g_bc = P.sbuf("gbc", [128, 4, D], F32)
    for r in range(4):
        P.dma("sync", g_bc[:, r, :], gains[r:r + 1, :].partition_broadcast(128), writes=[g_bc], track=g_bc)
    wo_sb = P.sbuf("wo", [128, 8, D], BF16)
    pg_sb = P.sbuf("pg", [128, 8, D], BF16)
    pp_sb = P.sbuf("pp", [128, 2, D], BF16)
    P.dma("pool", wo_sb[:, :, :], w_out[:, :].rearrange("(kc p) n -> p kc n", p=128), writes=[wo_sb], track=wo_sb)
    P.dma("pool", pg_sb[:, :, :], ple_wg[:, :].rearrange("(kc p) n -> p kc n", p=128), writes=[pg_sb], track=pg_sb)
    P.dma("pool", pp_sb[:, :, :], ple_wp[:, :].rearrange("(kc p) n -> p kc n", p=128), writes=[pp_sb], track=pp_sb)
    if kind == "moe":
        rw32 = P.sbuf("rw32", [128, 8, 8], F32)
        P.dma("sync", rw32[:, :, :], rwt[:, :].rearrange("(kc p) e -> p kc e", p=128), writes=[rw32], track=rw32)
        id32 = P.sbuf("id32", [128, 128], F32)
        P.dma("sync", id32[:, :], ident[:, :], writes=[id32], track=id32)

    xin = [P.sbuf("xin", [128, D], F32) for _ in range(2)]
    oin = [P.sbuf("oin", [128, D], F32) for _ in range(2)]
    pTs = [P.sbuf("pTs", [128, 2, 512], BF16) for _ in range(2)]
    sq = P.sbuf("sq", [128, D], BF16)
    ss = P.sbuf("ss", [128, 4], F32)
    rstd = P.sbuf("rstd", [128, 4], F32)
    hn = [P.sbuf("hn", [128, D], BF16) for _ in range(2)]
    tp = P.psum("tp", [128, D], BF16)
    mT = [P.sbuf("mT", [128, 8, 128], BF16) for _ in range(2)]
    hT = P.sbuf("hT", [128, 8, 512], BF16)
    x1 = P.sbuf("x1", [128, 4, D], F32)
    big = P.psum("big", [128, D], F32)
    gps = [P.psum("gps", [128, 512], F32) for _ in range(2)]
    ups = [P.psum("ups", [128, 512], F32) for _ in range(2)]
    sg = [P.sbuf("sg", [128, 512], BF16) for _ in range(2)]
    aT = [P.sbuf("aT", [128, 4, 512], BF16) for _ in range(2)]
    wg_b = [P.sbuf("wgb", [128, 8, 512], BF16) for _ in range(2)]
    wu_b = [P.sbuf("wub", [128, 8, 512], BF16) for _ in range(2)]
    wd_b = [P.sbuf("wdb", [128, 4, D], BF16) for _ in range(2)]
    gate_sb = P.sbuf("gate", [128, D], F32)
    tmp = P.sbuf("tmp", [128, D], F32)
    ot = [P.sbuf("ot", [128, D], F32) for _ in range(2)]
    if kind == "moe":
        hn32 = P.sbuf("hn32", [128, D], F32)
        logit = P.sbuf("logit", [128, 8], F32)
        mx8 = P.sbuf("mx8", [128, 8], F32)
        dd = P.sbuf("dd", [128, 2], F32)
        sgm = P.sbuf("sgm", [128, 2], F32)
        t1 = P.sbuf("t1", [128, 8], F32)
        gfull = P.sbuf("gfull", [128, 4, 8], F32)
        hT32 = P.sbuf("hT32", [128, 8, 128], F32)

    def mm_tok(lhsT_fn, rhs_fn, nk, rd):
        for half in range(2):
            for kc in range(nk):
                P.I("pe", "matmul", big[:, half * 512:(half + 1) * 512], lhsT=lhsT_fn(kc), rhs=rhs_fn(kc, half),
                    start=(kc == 0), stop=(kc == nk - 1), reads=rd, writes=[big])

    NT = tok // 512
    wcount = 0
    for T_ in range(NT):
        pb = pTs[T_ % 2]
        P.dma("pool", pb[:, :, :], pT[:, T_ * 512:(T_ + 1) * 512].rearrange("(kc p) n -> p kc n", p=128),
              writes=[pb], track=pb)
        for s in range(4):
            r0 = T_ * 512 + s * 128
            xi, oi = xin[s % 2], oin[s % 2]
            P.dma("sync", xi[:, :], x[r0:r0 + 128, :], writes=[xi], track=xi)
            P.dma("sync", oi[:, 0:512], oa[r0:r0 + 128, :], writes=[oi], track=oi)
            P.dma("sync", oi[:, 512:1024], orc[r0:r0 + 128, :], writes=[oi], track=oi)
            h_ = hn[0]
            emit_norm(P, sq, ss, rstd, 0, oi[:, 0:512], 512, g_bc[:, 0, 0:512], h_[:, 0:512], [oi], g_bc, h_)
            emit_norm(P, sq, ss, rstd, 1, oi[:, 512:1024], 512, g_bc[:, 0, 512:1024], h_[:, 512:1024], [oi], g_bc, h_)
            m_ = mT[s % 2]
            emit_transpose8(P, tp, id_bf, h_, m_[:, :, :], m_)
            mm_tok(lambda kc: m_[:, kc, :], lambda kc, half: wo_sb[:, kc, half * 512:(half + 1) * 512], 8, [m_, wo_sb])
            P.I("dve", "tensor_tensor", out=x1[:, s, :], in0=big[:, :], in1=xi[:, :], op=ALU.add,
                reads=[big, xi], writes=[x1])
            h2 = hn[1]
            if kind == "moe":
                emit_norm(P, sq, ss, rstd, 2, x1[:, s, :], D, g_bc[:, 1, :], h2[:, :], [x1], g_bc, h2, extra32=hn32)
                for kc in range(8):
                    P.I("pe", "transpose", out=big[:, kc * 128:(kc + 1) * 128], in_=hn32[:, kc * 128:(kc + 1) * 128],
                        identity=id32[:, :], reads=[hn32, id32], writes=[big])
                P.I("act", "copy", out=hT32[:, :, :], in_=big[:, :].rearrange("p (a b) -> p a b", a=8),
                    reads=[big], writes=[hT32])
                for kc in range(8):
                    P.I("pe", "matmul", gps[0][:, 0:8], lhsT=hT32[:, kc, :], rhs=rw32[:, kc, :],
                        start=(kc == 0), stop=(kc == 7), reads=[hT32, rw32], writes=[gps[0]])
                P.I("dve", "tensor_copy", out=logit[:, :], in_=gps[0][:, 0:8], reads=[gps[0]], writes=[logit])
                P.I("dve", "max", out=mx8[:, :], in_=logit[:, :], reads=[logit], writes=[mx8])
                P.I("dve", "tensor_tensor", out=dd[:, 0:1], in0=mx8[:, 0:1], in1=mx8[:, 1:2], op=ALU.subtract,
                    reads=[mx8], writes=[dd])
                P.I("act", "activation", out=sgm[:, 0:1], in_=dd[:, 0:1], func=AF.Sigmoid, reads=[dd], writes=[sgm])
                P.I("act", "activation", out=sgm[:, 1:2], in_=dd[:, 0:1], func=AF.Sigmoid, scale=-1.0,
                    reads=[dd], writes=[sgm])
                P.I("dve", "tensor_scalar", out=t1[:, :], in0=logit[:, :], scalar1=mx8[:, 0:1], scalar2=sgm[:, 0:1],
                    op0=ALU.is_equal, op1=ALU.mult, reads=[logit, mx8, sgm], writes=[t1])
                P.I("dve", "tensor_scalar", out=gfull[:, s, :], in0=logit[:, :], scalar1=mx8[:, 1:2],
                    scalar2=sgm[:, 1:2], op0=ALU.is_equal, op1=ALU.mult, reads=[logit, mx8, sgm], writes=[gfull])
                P.I("dve", "tensor_tensor", out=gfull[:, s, :], in0=gfull[:, s, :], in1=t1[:, :], op=ALU.add,
                    reads=[gfull, t1], writes=[gfull])
            else:
                emit_norm(P, sq, ss, rstd, 2, x1[:, s, :], D, g_bc[:, 1, :], h2[:, :], [x1], g_bc, h2)
            emit_transpose8(P, tp, id_bf, h2, hT[:, :, s * 128:(s + 1) * 128], hT)
        for e_ in range(NE):
            for (f0, fn_) in groups:
                wb = wcount % 2
                wcount += 1
                nch = fn_ // 128
                wgb, wub, wdb, a_ = wg_b[wb], wu_b[wb], wd_b[wb], aT[wb]
                P.dma("pool", wgb[:, :, 0:fn_], wg[e_, :, f0:f0 + fn_].rearrange("(kc p) n -> p kc n", p=128),
                      writes=[wgb], track=wgb)
                P.dma("pool", wub[:, :, 0:fn_], wu[e_, :, f0:f0 + fn_].rearrange("(kc p) n -> p kc n", p=128),
                      writes=[wub], track=wub)
                P.dma("pool", wdb[:, 0:nch, :], wd[e_, f0:f0 + fn_, :].rearrange("(c p) n -> p c n", p=128),
                      writes=[wdb], track=wdb)
                for c in range(nch):
                    gp, up, sg_ = gps[c % 2], ups[c % 2], sg[c % 2]
                    for kc in range(8):
                        P.I("pe", "matmul", gp[:, :], lhsT=wgb[:, kc, c * 128:(c + 1) * 128], rhs=hT[:, kc, :],
                            start=(kc == 0), stop=(kc == 7), reads=[wgb, hT], writes=[gp])
                    for kc in range(8):
                        P.I("pe", "matmul", up[:, :], lhsT=wub[:, kc, c * 128:(c + 1) * 128], rhs=hT[:, kc, :],
                            start=(kc == 0), stop=(kc == 7), reads=[wub, hT], writes=[up])
                    P.I("act", "activation", out=sg_[:, :], in_=gp[:, :], func=AF.Silu, reads=[gp], writes=[sg_])
                    P.I("dve", "tensor_tensor", out=a_[:, c, :], in0=up[:, :], in1=sg_[:, :], op=ALU.mult,
                        reads=[up, sg_], writes=[a_])
                for s in range(4):
                    mm_tok(lambda kc: a_[:, kc, s * 128:(s + 1) * 128],
                           lambda kc, half: wdb[:, kc, half * 512:(half + 1) * 512], nch, [a_, wdb])
                    if kind == "moe":
                        P.I("dve", "scalar_tensor_tensor", out=x1[:, s, :], in0=big[:, :], scalar=gfull[:, s, e_:e_ + 1],
                            in1=x1[:, s, :], op0=ALU.mult, op1=ALU.add, reads=[big, gfull, x1], writes=[x1])
                    else:
                        P.I("dve", "tensor_tensor", out=x1[:, s, :], in0=big[:, :], in1=x1[:, s, :], op=ALU.add,
                            reads=[big, x1], writes=[x1])
        for s in range(4):
            r0 = T_ * 512 + s * 128
            h_ = hn[s % 2]
            emit_norm(P, sq, ss, rstd, 3, x1[:, s, :], D, g_bc[:, 2, :], h_[:, :], [x1], g_bc, h_)
            m_ = mT[s % 2]
            emit_transpose8(P, tp, id_bf, h_, m_[:, :, :], m_)
            mm_tok(lambda kc: m_[:, kc, :], lambda kc, half: pg_sb[:, kc, half * 512:(half + 1) * 512], 8, [m_, pg_sb])
            P.I("act", "activation", out=gate_sb[:, :], in_=big[:, :], func=AF.Sigmoid, reads=[big], writes=[gate_sb])
            mm_tok(lambda kc: pb[:, kc, s * 128:(s + 1) * 128],
                   lambda kc, half: pp_sb[:, kc, half * 512:(half + 1) * 512], 2, [pb, pp_sb])
            P.I("dve", "tensor_tensor", out=tmp[:, :], in0=big[:, :], in1=gate_sb[:, :], op=ALU.mult,
                reads=[big, gate_sb], writes=[tmp])
            o_ = ot[s % 2]
            if final:
                P.I("dve", "tensor_tensor", out=x1[:, s, :], in0=x1[:, s, :], in1=tmp[:, :], op=ALU.add,
                    reads=[x1, tmp], writes=[x1])
                emit_norm(P, sq, ss, rstd, 0, x1[:, s, :], D, g_bc[:, 3, :], o_[:, :], [x1], g_bc, o_)
            else:
                P.I("dve", "tensor_tensor", out=o_[:, :], in0=x1[:, s, :], in1=tmp[:, :], op=ALU.add,
                    reads=[x1, tmp], writes=[o_])
            P.dma("sync", out[r0:r0 + 128, :], o_[:, :], reads=[o_], track=o_)
    return P.finish()


def consts_l2a(seq):
    n_cmp = seq // 16 - 1
    nct = seq // 2048
    c = {}
    c["ident"] = np.eye(128, dtype=np.float32)
    c["eall"] = (np.arange(128)[:, None] == (np.arange(seq)[None, :] // 64)).astype(np.float32)
    k = np.arange(128)[:, None]
    q = np.arange(128)[None, :]
    c["tri"] = np.where(k <= q, 0.0, NEG).astype(np.float32)
    c["wold"] = np.where(k > q, 0.0, NEG).astype(np.float32)
    cl = np.arange(128)[:, None, None]
    dl = np.arange(17)[None, :, None]
    ql = np.arange(128)[None, None, :]
    c["cm"] = np.where(128 * dl + ql >= 16 * cl + 31, 0.0, NEG).astype(np.float32)
    qq = np.arange(128)[:, None]
    dd = np.arange(256)[None, :] - 127
    hi = (qq >= 64).astype(np.int64)
    forced = (dd == hi) | (dd == hi - 1)
    invalid = dd > hi
    c["ftab"] = np.where(forced, 100.0, np.where(invalid, -100.0, 0.0)).astype(np.float32)
    kcc = np.zeros((4, nct * 128), np.float32)
    pos = 16 * np.arange(n_cmp) + 31
    kcc[0, :n_cmp] = pos // 64
    kcc[1, :n_cmp] = pos % 64
    kcc[2, :n_cmp] = 1.0
    kcc[3, :n_cmp] = 1.0
    c["kcc"] = kcc
    vones = np.ones((128, nct), np.float32)
    vones[127, nct - 1] = 0.0
    c["vones"] = vones
    A = np.zeros((nct * 128, 129), np.float32)
    cc = np.arange(n_cmp)
    np.add.at(A, (cc, cc // 4), 1.0)
    np.add.at(A, (cc, (cc + 1) // 4), 1.0)
    A[:n_cmp, 128] = 1.0
    c["aaug"] = np.ascontiguousarray(A.reshape(nct, 128, 129).transpose(1, 0, 2))
    return c


def alibi_rows_q(g, seq):
    t = np.arange(seq)
    rows = np.zeros((4, 4, seq), np.float32)
    for hg in range(4):
        s = 2.0 ** (-(4 * g + hg + 1))
        rows[0, hg] = 512.0 * s
        rows[1, hg] = 8.0 * s
        rows[2, hg] = -512.0 * s * (t // 64)
        rows[3, hg] = -8.0 * s * (t % 64)
    return rows


def key_rows(seq):
    t = np.arange(seq)
    return np.stack([t // 64, t % 64, np.ones(seq), np.ones(seq)]).astype(np.float32)


def inputs_l2a(proj_b, g, cmp_pos, cmp_w1, cmp_w2, consts):
    seq = proj_b.shape[0]
    m = dict(consts)
    q = proj_b[:, 0:512].reshape(seq, 8, 64)[:, 4 * g:4 * g + 4, :]
    m["qa"] = np.concatenate([q.transpose(2, 1, 0), alibi_rows_q(g, seq)], axis=0)
    sl = lambda o: proj_b[:, o + 64 * g:o + 64 * (g + 1)]
    kr = key_rows(seq)
    ones = np.ones((seq, 1), np.float32)
    m["kcT"] = np.ascontiguousarray(sl(512).T)
    m["vcT"] = np.ascontiguousarray(sl(640).T)
    m["kas"] = np.concatenate([sl(768).T, kr], axis=0)
    m["vas"] = np.concatenate([sl(896), ones], axis=1)
    m["kaw"] = np.concatenate([sl(1024).T, kr], axis=0)
    m["vaw"] = np.concatenate([sl(1152), ones], axis=1)
    m["gates"] = np.ascontiguousarray(proj_b[:, 1280 + 12 * g:1280 + 12 * (g + 1)])
    for kv, nm in enumerate(("k", "v")):
        m["w1" + nm] = np.ascontiguousarray(cmp_w1[kv].reshape(32, 64, 128).transpose(1, 0, 2))
        m["w2" + nm] = np.ascontiguousarray(cmp_w2[kv])
        m["pos" + nm] = np.ascontiguousarray(cmp_pos[kv].T)
    return {k_: np.ascontiguousarray(v, dtype=np.float32) for k_, v in m.items()}


def inputs_l2b(proj_b, g, conv_w, conv_b, wa, ba, wx, bx, lam):
    c0 = 256 * g
    m = {}
    m["xrT"] = np.ascontiguousarray(proj_b[:, 1304 + c0:1304 + c0 + 256].T)
    m["ygT"] = np.ascontiguousarray(proj_b[:, 1816 + c0:1816 + c0 + 256].T)
    pv = np.zeros((128, 2, 8), np.float32)
    wab = np.zeros((128, 2, 128), np.float32)
    wxb = np.zeros((128, 2, 128), np.float32)
    for ct in range(2):
        ch = slice(c0 + ct * 128, c0 + (ct + 1) * 128)
        pv[:, ct, 0:4] = conv_w[:, ch].T
        pv[:, ct, 4] = conv_b[ch]
        pv[:, ct, 5] = ba[ch]
        pv[:, ct, 6] = bx[ch]
        pv[:, ct, 7] = lam[ch]
        for k_ in range(2):
            n = (c0 + ct * 128) // 64 + k_
            wab[k_ * 64:(k_ + 1) * 64, ct, k_ * 64:(k_ + 1) * 64] = wa[n]
            wxb[k_ * 64:(k_ + 1) * 64, ct, k_ * 64:(k_ + 1) * 64] = wx[n]
    m["pv"], m["wab"], m["wxb"] = pv, wab, wxb
    return m


def kernel(**inp):
    f32 = lambda a: np.ascontiguousarray(np.asarray(a), dtype=np.float32)
    x = f32(inp["x"]).reshape(4 * SEQ, D)
    ident = np.eye(128, dtype=np.float32)
    cs = consts_l2a(SEQ)
    for li in range(2):
        last = (li == 1)
        nc = build_l1()
        w_in, gn = f32(inp["w_in"][li]), f32(inp["attn_norm"][li])[None, :]
        res = run_spmd(nc, [{"x": x[c * TOK:(c + 1) * TOK], "w": w_in, "g": gn, "ident": ident} for c in range(NCORES)])
        proj = np.concatenate([r["proj"] for r in res], axis=0)
        del res
        nc = build_l2a()
        cp, c1, c2 = f32(inp["cmp_pos"][li]), f32(inp["cmp_w1"][li]), f32(inp["cmp_w2"][li])
        maps = [inputs_l2a(proj[(c // 2) * SEQ:(c // 2 + 1) * SEQ], c % 2, cp, c1, c2, cs) for c in range(NCORES)]
        res = run_spmd(nc, maps)
        del maps
        oa = np.empty((4 * SEQ, 512), np.float32)
        for c in range(NCORES):
            oa[(c // 2) * SEQ:(c // 2 + 1) * SEQ, 256 * (c % 2):256 * (c % 2 + 1)] = res[c]["oat"]
        del res
        nc = build_l2b()
        lw = [f32(inp[k][li]) for k in ("conv_w", "conv_b", "lru_wa", "lru_ba", "lru_wx", "lru_bx", "lru_lambda")]
        maps = [inputs_l2b(proj[(c // 2) * SEQ:(c // 2 + 1) * SEQ], c % 2, *lw) for c in range(NCORES)]
        res = run_spmd(nc, maps)
        del maps, proj
        orc = np.empty((4 * SEQ, 512), np.float32)
        for c in range(NCORES):
            orc[(c // 2) * SEQ:(c // 2 + 1) * SEQ, 256 * (c % 2):256 * (c % 2 + 1)] = res[c]["orT"].T
        del res
        kind = "dense" if li % 2 == 0 else "moe"
        nc = build_l3(kind, last)
        gains = np.stack([np.concatenate([f32(inp["out_norm_attn"][li]), f32(inp["out_norm_rec"][li])]),
                          f32(inp["ffn_norm"][li]), f32(inp["ple_norm"][li]), f32(inp["final_norm"])])
        j = li // 2
        if kind == "dense":
            wg, wu, wd = f32(inp["dense_w_gate"][j])[None], f32(inp["dense_w_up"][j])[None], f32(inp["dense_w_down"][j])[None]
        else:
            wg, wu, wd = f32(inp["moe_w_gate"][j]), f32(inp["moe_w_up"][j]), f32(inp["moe_w_down"][j])
        p = f32(inp["p"][li]).reshape(4 * SEQ, 256)
        maps = []
        for c in range(NCORES):
            r = slice(c * TOK, (c + 1) * TOK)
            m = {"x": x[r], "oa": oa[r], "orc": orc[r], "pT": np.ascontiguousarray(p[r].T), "gains": gains,
                 "w_out": f32(inp["w_out"][li]), "ple_wg": f32(inp["ple_w_gate"][li]), "ple_wp": f32(inp["ple_w_proj"][li]),
                 "ident": ident, "wg": wg, "wu": wu, "wd": wd}
            if kind == "moe":
                m["rwt"] = f32(inp["router_w"][j])
            maps.append(m)
        res = run_spmd(nc, maps)
        del maps
        x = np.concatenate([r["out"] for r in res], axis=0)
        del res
    return x.reshape(4, SEQ, D)
```
